# Optimizing a Trainium2 kernel written in Bass

```python
import jax, jax.numpy as jnp
from jax import lax
import numpy as np

D_MODEL = 1024
BATCH = 1
SEQ = 16384
DEPTH = 2

N_META = 16
N_A_LAYERS = DEPTH // 2
N_B_LAYERS = DEPTH - N_A_LAYERS
CONV_W = 3
N_HEADS = 8
QK_NOPE_DIM = 128
QK_ROPE_DIM = 64
V_DIM = 128
Q_RANK = 384
KV_RANK = 256
ROPE_THETA = 10000.0
Q_BLOCK = 128
N_GROUPS = 8
EXPERTS_PER_GROUP = 8
N_EXPERTS = N_GROUPS * EXPERTS_PER_GROUP
TOP_K_IN_GROUP = 2
D_EXPERT = 256
EXPERT_BLOCK = 128
NORM_EPS = 1e-6

kernel_name = 'yoco_shortconv_mla_hier_moe'


def rmsnorm(x, g):
    xf = x.astype(jnp.float32)
    y = xf * lax.rsqrt(jnp.mean(xf * xf, axis=-1, keepdims=True) + NORM_EPS)
    return (y * g.astype(jnp.float32)).astype(x.dtype)


def rope_tables(t):
    inv = ROPE_THETA ** (-jnp.arange(0, QK_ROPE_DIM, 2, dtype=jnp.float32) / QK_ROPE_DIM)
    ang = jnp.arange(t, dtype=jnp.float32)[:, None] * inv[None, :]
    return jnp.cos(ang), jnp.sin(ang)


def apply_rope(x, cos, sin):
    half = x.shape[-1] // 2
    x1, x2 = x[..., :half], x[..., half:]
    cos = cos.astype(x.dtype)
    sin = sin.astype(x.dtype)
    return jnp.concatenate([x1 * cos - x2 * sin, x1 * sin + x2 * cos], axis=-1)


def short_conv_mixer(hn, w_in, conv_w, w_out):
    bcu = hn @ w_in
    b, c, u = jnp.split(bcu, 3, axis=-1)
    z = c * u
    z = lax.conv_general_dilated(z, conv_w.astype(z.dtype), window_strides=(1,),
                                 padding=((CONV_W - 1, 0),),
                                 dimension_numbers=('NWC', 'WIO', 'NWC'),
                                 feature_group_count=z.shape[-1])
    return (b * z) @ w_out


def shared_kv_side(h, kv_g, w_dkv, ckv_g, w_ukv, cos, sin):
    bsz, t, _ = h.shape
    a = rmsnorm(h, kv_g) @ w_dkv
    c_kv = rmsnorm(a[..., :KV_RANK], ckv_g)
    k_rope = apply_rope(a[..., KV_RANK:], cos, sin)
    kv = (c_kv @ w_ukv).reshape(bsz, t, N_HEADS, QK_NOPE_DIM + V_DIM)
    return kv[..., :QK_NOPE_DIM], k_rope, kv[..., QK_NOPE_DIM:]


def causal_block_attention(q_nope, q_rope, k_nope, k_rope, v):
    bsz, t, nh, _ = q_nope.shape
    nqb = -(-t // Q_BLOCK)
    pad = nqb * Q_BLOCK - t
    def blocks(q):
        q = jnp.pad(q, ((0, 0), (0, pad), (0, 0), (0, 0)))
        return q.reshape(bsz, nqb, Q_BLOCK, nh, q.shape[-1]).transpose(1, 0, 2, 3, 4)
    qn, qr = blocks(q_nope), blocks(q_rope)
    kpos = jnp.arange(t)
    scale = (QK_NOPE_DIM + QK_ROPE_DIM) ** -0.5
    def one_block(args):
        qn_b, qr_b, j = args
        qpos = j * Q_BLOCK + jnp.arange(Q_BLOCK)
        s = (jnp.einsum('bqhd,bkhd->bhqk', qn_b, k_nope)
             + jnp.einsum('bqhd,bkd->bhqk', qr_b, k_rope)).astype(jnp.float32) * scale
        s = jnp.where(kpos[None, :] <= qpos[:, None], s, -jnp.inf)
        p = jax.nn.softmax(s, axis=-1).astype(v.dtype)
        return jnp.einsum('bhqk,bkhd->bqhd', p, v)
    o = lax.map(one_block, (qn, qr, jnp.arange(nqb, dtype=jnp.int32)))
    o = o.transpose(1, 0, 2, 3, 4).reshape(bsz, nqb * Q_BLOCK, nh * v.shape[-1])
    return o[:, :t]


def mla_mixer(hn, w_dq, cq_g, w_uq, w_o, k_nope, k_rope, v, cos, sin):
    bsz, t, _ = hn.shape
    cq = rmsnorm(hn @ w_dq, cq_g)
    q = (cq @ w_uq).reshape(bsz, t, N_HEADS, QK_NOPE_DIM + QK_ROPE_DIM)
    q_nope = q[..., :QK_NOPE_DIM]
    q_rope = apply_rope(q[..., QK_NOPE_DIM:], cos[:, None, :], sin[:, None, :])
    o = causal_block_attention(q_nope, q_rope, k_nope, k_rope, v)
    return o @ w_o


def hier_moe(hn, w_grp, w_exp, w_gate, w_up, w_down):
    bsz, t, d = hn.shape
    n = bsz * t
    xf = hn.reshape(n, d)
    g_logits = (xf @ w_grp).astype(jnp.float32)
    p_grp = jax.nn.softmax(g_logits, axis=-1)
    grp = jnp.argmax(g_logits, axis=-1).astype(jnp.int32)
    p_g = jnp.max(p_grp, axis=-1, keepdims=True)
    e_logits = (xf @ w_exp).astype(jnp.float32).reshape(n, N_GROUPS, EXPERTS_PER_GROUP)
    e_in = jnp.einsum('ng,nge->ne', jax.nn.one_hot(grp, N_GROUPS, dtype=jnp.float32), e_logits)
    top_p, top_i = lax.top_k(jax.nn.softmax(e_in, axis=-1), TOP_K_IN_GROUP)
    gate = p_g * top_p / jnp.sum(top_p, axis=-1, keepdims=True)
    expert = grp[:, None] * EXPERTS_PER_GROUP + top_i.astype(jnp.int32)
    a = n * TOP_K_IN_GROUP
    e_a = expert.reshape(a)
    w_a = gate.reshape(a).astype(hn.dtype)
    tok_a = jnp.repeat(jnp.arange(n, dtype=jnp.int32), TOP_K_IN_GROUP)
    counts = jax.ops.segment_sum(jnp.ones((a,), jnp.int32), e_a, num_segments=N_EXPERTS)
    padded = ((counts + EXPERT_BLOCK - 1) // EXPERT_BLOCK) * EXPERT_BLOCK
    pad_end = jnp.cumsum(padded)
    pad_start = pad_end - padded
    start = jnp.cumsum(counts) - counts
    order = jnp.argsort(e_a)
    e_s = e_a[order]
    dest = pad_start[e_s] + jnp.arange(a, dtype=jnp.int32) - start[e_s]
    n_blocks = (a + N_EXPERTS * (EXPERT_BLOCK - 1) + EXPERT_BLOCK - 1) // EXPERT_BLOCK
    p_slots = n_blocks * EXPERT_BLOCK
    slot_tok = jnp.full((p_slots,), n, jnp.int32).at[dest].set(tok_a[order])
    slot_w = jnp.zeros((p_slots,), hn.dtype).at[dest].set(w_a[order])
    blk_expert = jnp.minimum(jnp.searchsorted(pad_end, jnp.arange(n_blocks, dtype=jnp.int32) * EXPERT_BLOCK,
                                              side='right'), N_EXPERTS - 1).astype(jnp.int32)
    x_pad = jnp.concatenate([xf, jnp.zeros((1, d), xf.dtype)], axis=0)
    xs = x_pad[slot_tok].reshape(n_blocks, EXPERT_BLOCK, d)
    def expert_block(args):
        xb, e = args
        hh = jax.nn.silu(xb @ w_gate[e]) * (xb @ w_up[e])
        return hh @ w_down[e]
    ys = lax.map(expert_block, (xs, blk_expert)).reshape(p_slots, d)
    y = jnp.zeros((n + 1, d), ys.dtype).at[slot_tok].add(ys * slot_w[:, None])[:n]
    return y.reshape(bsz, t, d)


def setup_inputs(seed: int = 0) -> dict:
    key = jax.random.key(seed)
    ks = jax.random.split(key, 21)
    def nrm(k, shape, scale):
        return jax.random.normal(k, shape, jnp.float32) * scale
    d = D_MODEL
    return {
        'x': nrm(ks[0], (BATCH, SEQ, d), 1.0),
        'meta_tokens': nrm(ks[1], (N_META, d), 1.0),
        'norm_mix_g': 1.0 + nrm(ks[2], (DEPTH, d), 0.02),
        'norm_ffn_g': 1.0 + nrm(ks[3], (DEPTH, d), 0.02),
        'conv_w_in': nrm(ks[4], (N_A_LAYERS, d, 3 * d), d ** -0.5),
        'conv_w': nrm(ks[5], (N_A_LAYERS, CONV_W, 1, d), CONV_W ** -0.5),
        'conv_w_out': nrm(ks[6], (N_A_LAYERS, d, d), d ** -0.5),
        'kv_norm_g': 1.0 + nrm(ks[7], (d,), 0.02),
        'w_dkv': nrm(ks[8], (d, KV_RANK + QK_ROPE_DIM), d ** -0.5),
        'ckv_norm_g': 1.0 + nrm(ks[9], (KV_RANK,), 0.02),
        'w_ukv': nrm(ks[10], (KV_RANK, N_HEADS * (QK_NOPE_DIM + V_DIM)), KV_RANK ** -0.5),
        'w_dq': nrm(ks[11], (N_B_LAYERS, d, Q_RANK), d ** -0.5),
        'cq_norm_g': 1.0 + nrm(ks[12], (N_B_LAYERS, Q_RANK), 0.02),
        'w_uq': nrm(ks[13], (N_B_LAYERS, Q_RANK, N_HEADS * (QK_NOPE_DIM + QK_ROPE_DIM)), Q_RANK ** -0.5),
        'w_o': nrm(ks[14], (N_B_LAYERS, N_HEADS * V_DIM, d), (N_HEADS * V_DIM) ** -0.5),
        'w_grp': nrm(ks[15], (DEPTH, d, N_GROUPS), d ** -0.5),
        'w_exp': nrm(ks[16], (DEPTH, d, N_EXPERTS), d ** -0.5),
        'w_gate': nrm(ks[17], (DEPTH, N_EXPERTS, d, D_EXPERT), d ** -0.5),
        'w_up': nrm(ks[18], (DEPTH, N_EXPERTS, d, D_EXPERT), d ** -0.5),
        'w_down': nrm(ks[19], (DEPTH, N_EXPERTS, D_EXPERT, d), D_EXPERT ** -0.5),
        'final_norm_g': 1.0 + nrm(ks[20], (d,), 0.02),
    }


def reference(x, meta_tokens, norm_mix_g, norm_ffn_g, conv_w_in, conv_w, conv_w_out,
              kv_norm_g, w_dkv, ckv_norm_g, w_ukv, w_dq, cq_norm_g, w_uq, w_o,
              w_grp, w_exp, w_gate, w_up, w_down, final_norm_g):
    bsz = x.shape[0]
    meta = jnp.broadcast_to(meta_tokens[None].astype(x.dtype), (bsz, N_META, x.shape[-1]))
    h = jnp.concatenate([meta, x], axis=1)
    cos, sin = rope_tables(h.shape[1])
    shared = None
    for i in range(DEPTH):
        if i < N_A_LAYERS:
            h = h + short_conv_mixer(rmsnorm(h, norm_mix_g[i]), conv_w_in[i], conv_w[i], conv_w_out[i])
        else:
            if shared is None:
                shared = shared_kv_side(h, kv_norm_g, w_dkv, ckv_norm_g, w_ukv, cos, sin)
            j = i - N_A_LAYERS
            k_nope, k_rope, v = shared
            h = h + mla_mixer(rmsnorm(h, norm_mix_g[i]), w_dq[j], cq_norm_g[j], w_uq[j], w_o[j],
                              k_nope, k_rope, v, cos, sin)
        h = h + hier_moe(rmsnorm(h, norm_ffn_g[i]), w_grp[i], w_exp[i], w_gate[i], w_up[i], w_down[i])
    return rmsnorm(h, final_norm_g)[:, N_META:]
```

```python
import sys
import numpy as np
from contextlib import ExitStack
import concourse.bass as bass
import concourse.mybir as mybir
from concourse.bass_utils import run_bass_kernel_spmd

F32 = mybir.dt.float32
BF16 = mybir.dt.bfloat16
AF = mybir.ActivationFunctionType
ALU = mybir.AluOpType
AX = mybir.AxisListType

NCORES = 8
D = 1024
SEQ = 16384
NMETA = 16
NBLK = 16
NTOK = NBLK * 128
NT_A = NTOK + NMETA
TKEYS = SEQ + NMETA
NH = 8
NEXP = 64
DEXP = 256
EPS = 1e-6
SCALE = float((128 + 64) ** -0.5)
NEG = -30000.0


class Prog:
    ENG = ['pe', 'act', 'dve', 'pool', 'sp']

    def __init__(self, nc):
        self.nc = nc
        self.es = ExitStack()
        self.sems = {}
        self.cnt = {e: 0 for e in self.ENG}
        self.dcnt = {}
        self._reset()

    def _reset(self):
        self.ops = []
        self.last_w = {}
        self.readers = {}
        self.dma_hist = {}

    def _sem(self, sk):
        if sk not in self.sems:
            self.sems[sk] = self.es.enter_context(self.nc.semaphore("s%d" % len(self.sems)))
        return self.sems[sk]

    def _add(self, eng, fn, reads, writes, dma_key=None):
        idx = len(self.ops)
        deps = set()
        for r in reads:
            if r in self.last_w:
                deps.add(self.last_w[r])
        for w in writes:
            if w in self.last_w:
                deps.add(self.last_w[w])
            for rd in self.readers.get(w, ()):
                deps.add(rd)
        for w in writes:
            self.last_w[w] = idx
            self.readers[w] = []
        for r in reads:
            if r not in writes:
                self.readers.setdefault(r, []).append(idx)
        self.ops.append(dict(eng=eng, fn=fn, deps=sorted(deps), dma_key=dma_key, users=0, sig=None))
        return idx

    def op(self, eng, fn, reads=(), writes=()):
        return self._add(eng, fn, tuple(reads), tuple(writes))

    def dma(self, eng, fn, reads=(), writes=(), key=None):
        return self._add(eng, fn, tuple(reads), tuple(writes), dma_key=key)

    @staticmethod
    def _skip(do, o):
        return do['dma_key'] is None and o['dma_key'] is None and do['eng'] == 'pe' and o['eng'] == 'pe'

    def emit(self, final=False):
        nc = self.nc
        ops = self.ops
        for o in ops:
            for d in o['deps']:
                if not self._skip(ops[d], o):
                    ops[d]['users'] += 1
        for i, o in enumerate(ops):
            if o['dma_key'] is not None:
                k = o['dma_key']
                self.dcnt[k] = self.dcnt.get(k, 0) + 16
                o['sig'] = ('d', k, self.dcnt[k])
                self.dma_hist.setdefault(k, []).append((i, self.dcnt[k]))
                self._sem(('d', k))
            elif o['users'] > 0:
                self.cnt[o['eng']] += 1
                o['sig'] = ('e', o['eng'], self.cnt[o['eng']])
                self._sem(('e', o['eng']))
        per = {e: [] for e in self.ENG}
        for i, o in enumerate(ops):
            per[o['eng']].append(i)
        print("phase ops", len(ops), {e: len(per[e]) for e in per}, "sems", len(self.sems), file=sys.stderr)
        dcnt_end = dict(self.dcnt)

        def body(ename, eobj):
            waited = {}
            for i in per[ename]:
                o = ops[i]
                need = {}
                for d in o['deps']:
                    do = ops[d]
                    if self._skip(do, o):
                        continue
                    s = do['sig']
                    sk = (s[0], s[1])
                    v = s[2]
                    if s[0] == 'd':
                        for (j, cv) in self.dma_hist[s[1]]:
                            if j < i:
                                v = max(v, cv)
                    need[sk] = max(need.get(sk, 0), v)
                for sk, v in need.items():
                    if waited.get(sk, 0) >= v:
                        continue
                    eobj.wait_ge(self.sems[sk], v)
                    waited[sk] = v
                ins = o['fn'](eobj)
                s = o['sig']
                if s is not None:
                    ins.then_inc(self.sems[(s[0], s[1])], 16 if s[0] == 'd' else 1)
            if ename == 'sp':
                for k, v in dcnt_end.items():
                    if waited.get(('d', k), 0) < v:
                        eobj.wait_ge(self.sems[('d', k)], v)

        with nc.Block() as block:
            @block.tensor
            def _(e):
                body('pe', e)

            @block.scalar
            def _(e):
                body('act', e)

            @block.vector
            def _(e):
                body('dve', e)

            @block.gpsimd
            def _(e):
                body('pool', e)

            @block.sync
            def _(e):
                body('sp', e)
        self._reset()
        if final:
            self.es.close()


class Ctx:
    pass


_UID = [0]


def _uname(n):
    _UID[0] += 1
    return "t%d_%s" % (_UID[0], n)


def new_stack(nc):
    es = ExitStack()
    sb = lambda n, s, d: es.enter_context(nc.sbuf_tensor(_uname(n), s, d))
    ps = lambda n, s, d: es.enter_context(nc.psum_tensor(_uname(n), s, d))
    return es, sb, ps


def load_bcast(P, eng, tile, dram_vec, name):
    n = dram_vec.shape[0]
    P.dma(eng, lambda e: e.dma_start(out=tile[:, :n], in_=dram_vec.partition_broadcast(128)), writes=[name], key=name)


def norm_rstd(P, C, src, n, width, tag):
    P.op('dve', lambda e: e.scalar_tensor_tensor(out=C.junk[:n, :width], in0=src, scalar=1.0, in1=src,
                                                  op0=ALU.mult, op1=ALU.mult, accum_out=C.ss[:n, :]),
         reads=(list(tag) if isinstance(tag, list) else [tag]), writes=['junk', 'ss'])
    P.op('act', lambda e: e.activation(out=C.rs[:n, :], in_=C.ss[:n, :], func=AF.Sqrt, scale=1.0 / width, bias=C.epsT[:n, :]),
         reads=['ss', 'eps'], writes=['rs'])
    P.op('dve', lambda e: e.reciprocal(out=C.rstd[:n, :], in_=C.rs[:n, :]), reads=['rs'], writes=['rstd'])


def norm_T(P, C, src, src_tag, n, g_bc, g_tag, dst16=None, dst16_tag=None, dst32=None, dst32_tag=None):
    norm_rstd(P, C, src, n, D, src_tag)
    P.op('dve', lambda e: e.scalar_tensor_tensor(out=C.hn[:n, :], in0=src, scalar=C.rstd[:n, :], in1=g_bc[:n, :],
                                                  op0=ALU.mult, op1=ALU.mult),
         reads=(list(src_tag) if isinstance(src_tag, list) else [src_tag]) + ['rstd', g_tag], writes=['hn'])
    for k in range(8):
        P.op('pe', lambda e, k=k: e.transpose(C.pT32[:, k, :n], C.hn[:n, k * 128:(k + 1) * 128], C.ident32[:n, :n]),
             reads=['hn', 'ident32'], writes=['pT32'])
    if dst16 is not None:
        P.op('act', lambda e: e.activation(out=dst16, in_=C.pT32[:, :, :n], func=AF.Copy), reads=['pT32'], writes=[dst16_tag])
    if dst32 is not None:
        P.op('dve', lambda e: e.tensor_copy(out=dst32, in_=C.pT32[:, :, :n]), reads=['pT32'], writes=[dst32_tag])


def alloc_norm(C, sb, ps):
    C.junk = sb("junk", [128, 1024], F32)
    C.hn = sb("hn", [128, 1024], F32)
    C.ss = sb("ss", [128, 1], F32)
    C.rs = sb("rs", [128, 1], F32)
    C.rstd = sb("rstd", [128, 1], F32)
    C.pT32 = ps("pT32", [128, 8, 128], F32)


def load_consts(P, C, sb, ident_d):
    C.ident32 = sb("ident32", [128, 128], F32)
    C.identb = sb("identb", [128, 128], BF16)
    C.epsT = sb("epsT", [128, 1], F32)
    P.dma('sp', lambda e: e.dma_start(out=C.ident32[:], in_=ident_d), writes=['ident32'], key='ident32')
    P.op('dve', lambda e: e.tensor_copy(out=C.identb[:], in_=C.ident32[:]), reads=['ident32'], writes=['identb'])
    P.op('dve', lambda e: e.memset(C.epsT[:], EPS), writes=['eps'])


def moe_phase(P, nc, C, h, blocks, w_router_d, gffn_d, wg_d, wu_d, wd_d, nexp=NEXP):
    es, sb, ps = new_stack(nc)
    alloc_norm(C, sb, ps)
    ntot = sum(n for _, n in blocks)
    offs = []
    o = 0
    for b, n in blocks:
        offs.append(o)
        o += n
    nb = len(blocks)
    hnT = sb("m_hnT", [128, 8, ntot], BF16)
    hlo = sb("m_hlo", [128, 8, 128], BF16)
    gbc = sb("m_gbc", [128, 1024], F32)
    wr = sb("m_wr", [128, 8, 72], F32)
    wrh = sb("m_wrh", [128, 8, 72], BF16)
    wrl = sb("m_wrl", [128, 8, 72], BF16)
    G = sb("m_G", [128, nb, 64], F32)
    lg = sb("m_lg", [128, nb, 72], F32)
    A8 = [sb("m_a8_%d" % i, [128, nb, 8], F32) for i in range(8)]
    A1 = [sb("m_a1_%d" % i, [128, nb], F32) for i in range(8)]
    t64 = sb("m_t64", [128, nb, 8, 8], F32)
    po_ = [ps("m_po%d" % i, [128, 512], F32) for i in range(2)]
    pr = po_[0]
    load_bcast(P, 'sp', gbc, gffn_d, 'm_gbc')
    P.dma('sp', lambda e: e.dma_start(out=wr[:], in_=w_router_d.rearrange("(k p) n -> p k n", p=128)), writes=['m_wr'], key='m_wr')
    P.op('dve', lambda e: e.tensor_copy(out=wrh[:], in_=wr[:]), reads=['m_wr'], writes=['m_wrh'])
    P.op('dve', lambda e: e.tensor_tensor(out=wrl[:], in0=wr[:], in1=wrh[:], op=ALU.subtract), reads=['m_wr', 'm_wrh'], writes=['m_wrl'])
    P.op('dve', lambda e: e.memset(lg[:], 0.0), writes=['lg'])
    for bi, (b, n) in enumerate(blocks):
        o = offs[bi]
        norm_T(P, C, h[:n, b, :], [('h', b), ('h', b, 0), ('h', b, 1)], n, gbc, 'm_gbc', dst16=hnT[:, :, o:o + n], dst16_tag=('m_hnT', bi))
        P.op('dve', lambda e, n=n, o=o: e.tensor_tensor(out=hlo[:, :, :n], in0=C.pT32[:, :, :n], in1=hnT[:, :, o:o + n], op=ALU.subtract),
             reads=['pT32', ('m_hnT', bi)], writes=['m_hlo'])
        seq = [(hnT, o, wrh), (hlo, 0, wrh), (hnT, o, wrl)]
        for si, (xt_, xo, wt_) in enumerate(seq):
            for k in range(8):
                P.op('pe', lambda e, k=k, n=n, xt_=xt_, xo=xo, wt_=wt_, si=si: e.matmul(pr[:n, :72], lhsT=xt_[:, k, xo:xo + n], rhs=wt_[:, k, :],
                                                                                     start=(si == 0 and k == 0), stop=(si == 2 and k == 7)),
                     reads=[('m_hnT', bi), 'm_hlo', 'm_wrh', 'm_wrl'], writes=[('po', 0)])
        P.op('dve', lambda e, n=n, bi=bi: e.tensor_copy(out=lg[:n, bi, :], in_=pr[:n, :72]), reads=[('po', 0)], writes=['lg'])
    gl = lg[:, :, 0:8]
    el = lg[:, :, 8:72].rearrange("p b (g e) -> p b g e", e=8)
    ohg, dd, ein, oh1, e2, oh2, gsel, tmp8 = [t[:, :, :] for t in A8]
    gmax, sg, pg, m1, m2, dm, ex, w2 = [t[:, :] for t in A1]
    b3 = lambda a: a.unsqueeze(2).broadcast_to([128, nb, 8])
    DV = lambda fn, r, w: P.op('dve', fn, reads=r, writes=w)
    DV(lambda e: e.tensor_reduce(out=gmax, in_=gl, axis=AX.X, op=ALU.max), ['lg'], ['gmax'])
    DV(lambda e: e.tensor_tensor(out=ohg, in0=gl, in1=b3(gmax), op=ALU.is_equal), ['lg', 'gmax'], ['ohg'])
    DV(lambda e: e.tensor_tensor(out=dd, in0=gl, in1=b3(gmax), op=ALU.subtract), ['lg', 'gmax'], ['dd'])
    P.op('act', lambda e: e.activation(out=dd, in_=dd, func=AF.Exp), reads=['dd'], writes=['dd'])
    DV(lambda e: e.tensor_reduce(out=sg, in_=dd, axis=AX.X, op=ALU.add), ['dd'], ['sg'])
    DV(lambda e: e.reciprocal(out=pg, in_=sg), ['sg'], ['pg'])
    DV(lambda e: e.tensor_tensor(out=t64[:], in0=el, in1=ohg.unsqueeze(3).broadcast_to([128, nb, 8, 8]), op=ALU.mult), ['lg', 'ohg'], ['t64'])
    DV(lambda e: e.tensor_reduce(out=ein, in_=t64[:].rearrange("p b g e -> p b e g"), axis=AX.X, op=ALU.add), ['t64'], ['ein'])
    DV(lambda e: e.tensor_reduce(out=m1, in_=ein, axis=AX.X, op=ALU.max), ['ein'], ['m1'])
    DV(lambda e: e.tensor_tensor(out=oh1, in0=ein, in1=b3(m1), op=ALU.is_equal), ['ein', 'm1'], ['oh1'])
    DV(lambda e: e.scalar_tensor_tensor(out=e2, in0=oh1, scalar=-1e30, in1=ein, op0=ALU.mult, op1=ALU.add), ['oh1', 'ein'], ['e2'])
    DV(lambda e: e.tensor_reduce(out=m2, in_=e2, axis=AX.X, op=ALU.max), ['e2'], ['m2'])
    DV(lambda e: e.tensor_tensor(out=oh2, in0=e2, in1=b3(m2), op=ALU.is_equal), ['e2', 'm2'], ['oh2'])
    DV(lambda e: e.tensor_tensor(out=dm, in0=m2, in1=m1, op=ALU.subtract), ['m1', 'm2'], ['dm'])
    P.op('act', lambda e: e.activation(out=ex, in_=dm, func=AF.Exp), reads=['dm'], writes=['ex'])
    DV(lambda e: e.tensor_scalar(out=ex, in0=ex, scalar1=1.0, scalar2=None, op0=ALU.add), ['ex'], ['ex'])
    DV(lambda e: e.reciprocal(out=dm, in_=ex), ['ex'], ['dm'])
    DV(lambda e: e.tensor_tensor(out=m1, in0=pg, in1=dm, op=ALU.mult), ['pg', 'dm', 'oh1'], ['m1'])
    DV(lambda e: e.tensor_tensor(out=w2, in0=pg, in1=m1, op=ALU.subtract), ['pg', 'm1'], ['w2'])
    DV(lambda e: e.tensor_tensor(out=tmp8, in0=oh1, in1=b3(m1), op=ALU.mult), ['oh1', 'm1'], ['tmp8'])
    DV(lambda e: e.tensor_tensor(out=gsel, in0=oh2, in1=b3(w2), op=ALU.mult), ['oh2', 'w2'], ['gsel'])
    DV(lambda e: e.tensor_tensor(out=gsel, in0=gsel, in1=tmp8, op=ALU.add), ['gsel', 'tmp8'], ['gsel'])
    DV(lambda e: e.tensor_tensor(out=G[:].rearrange("p b (g e) -> p b g e", e=8), in0=ohg.unsqueeze(3).broadcast_to([128, nb, 8, 8]),
                                 in1=gsel.unsqueeze(2).broadcast_to([128, nb, 8, 8]), op=ALU.mult),
       ['ohg', 'gsel'], [('G', bi) for bi in range(nb)])
    groups = []
    cur = []
    for bi, (b, n) in enumerate(blocks):
        if n == 128 and len(cur) < 4 and (not cur or blocks[cur[-1]][1] == 128):
            cur.append(bi)
        else:
            if cur:
                groups.append(cur)
            cur = [bi]
        if len(cur) == 4:
            groups.append(cur)
            cur = []
    if cur:
        groups.append(cur)
    NW = 2
    wg = [sb("m_wg%d" % i, [128, 8, 256], BF16) for i in range(NW)]
    wu = [sb("m_wu%d" % i, [128, 8, 256], BF16) for i in range(NW)]
    wd = [sb("m_wd%d" % i, [128, 2, 1024], BF16) for i in range(NW)]
    pg_ = [ps("m_pg%d" % i, [128, 512], F32) for i in range(2)]
    pu_ = [ps("m_pu%d" % i, [128, 512], F32) for i in range(2)]
    sgt = [sb("m_sg%d" % i, [128, 512], F32) for i in range(2)]
    hh = [sb("m_hh%d" % i, [128, 2, 512], BF16) for i in range(2)]
    it = 0
    ob = 0
    for ex_i in range(nexp):
        s = ex_i % NW
        P.dma('pool', lambda e, s=s, ex_i=ex_i: e.dma_start(out=wg[s][:], in_=wg_d[ex_i].rearrange("(k p) n -> p k n", p=128)),
              writes=[('wg', s)], key=('wg', s))
        P.dma('pool', lambda e, s=s, ex_i=ex_i: e.dma_start(out=wu[s][:], in_=wu_d[ex_i].rearrange("(k p) n -> p k n", p=128)),
              writes=[('wu', s)], key=('wu', s))
        P.dma('pool', lambda e, s=s, ex_i=ex_i: e.dma_start(out=wd[s][:], in_=wd_d[ex_i].rearrange("(k p) n -> p k n", p=128)),
              writes=[('wd', s)], key=('wd', s))
        for grp in groups:
            o0 = offs[grp[0]]
            gn = sum(blocks[bi][1] for bi in grp)
            hs = it % 2
            it += 1
            hh_reads = []
            for fc in range(2):
                ps_i = fc
                for k in range(8):
                    P.op('pe', lambda e, s=s, fc=fc, k=k, o0=o0, gn=gn, ps_i=ps_i: e.matmul(
                        pg_[ps_i][:, :gn], lhsT=wg[s][:, k, fc * 128:(fc + 1) * 128], rhs=hnT[:, k, o0:o0 + gn], start=(k == 0), stop=(k == 7)),
                        reads=[('wg', s)] + [('m_hnT', bi) for bi in grp], writes=[('pg', ps_i)])
                for k in range(8):
                    P.op('pe', lambda e, s=s, fc=fc, k=k, o0=o0, gn=gn, ps_i=ps_i: e.matmul(
                        pu_[ps_i][:, :gn], lhsT=wu[s][:, k, fc * 128:(fc + 1) * 128], rhs=hnT[:, k, o0:o0 + gn], start=(k == 0), stop=(k == 7)),
                        reads=[('wu', s)] + [('m_hnT', bi) for bi in grp], writes=[('pu', ps_i)])
                P.op('act', lambda e, fc=fc, gn=gn, ps_i=ps_i: e.activation(out=sgt[fc][:, :gn], in_=pg_[ps_i][:, :gn], func=AF.Silu),
                     reads=[('pg', ps_i)], writes=[('sgt', fc)])
                P.op('dve', lambda e, fc=fc, gn=gn, ps_i=ps_i, hs=hs: e.tensor_tensor(out=hh[hs][:, fc, :gn], in0=sgt[fc][:, :gn], in1=pu_[ps_i][:, :gn], op=ALU.mult),
                     reads=[('sgt', fc), ('pu', ps_i)], writes=[('hh', hs, fc)])
            for j, bi in enumerate(grp):
                b, n = blocks[bi]
                for half in range(2):
                    pi = ob % 2
                    ob += 1
                    for fc in range(2):
                        P.op('pe', lambda e, s=s, fc=fc, j=j, n=n, half=half, pi=pi, hs=hs: e.matmul(
                            po_[pi][:n, :], lhsT=hh[hs][:, fc, j * 128:j * 128 + n], rhs=wd[s][:, fc, half * 512:(half + 1) * 512],
                            start=(fc == 0), stop=(fc == 1)),
                            reads=[('wd', s), ('hh', hs, 0), ('hh', hs, 1)], writes=[('po', pi)])
                    P.op('dve', lambda e, b=b, n=n, half=half, pi=pi, bi=bi, ex_i=ex_i: e.scalar_tensor_tensor(
                        out=h[:n, b, half * 512:(half + 1) * 512], in0=po_[pi][:n, :], scalar=G[:n, bi, ex_i:ex_i + 1],
                        in1=h[:n, b, half * 512:(half + 1) * 512], op0=ALU.mult, op1=ALU.add),
                        reads=[('po', pi), ('G', bi), ('h', b, half)], writes=[('h', b, half)])
    P.emit()
    es.close()


def build_A(nexp=NEXP, stop_after=99):
    nc = bass.Bass("TRN2", target_bir_lowering=False)
    dt = lambda n, s, d=F32, k="ExternalInput": nc.dram_tensor(n, s, d, kind=k).ap()
    xa = dt("xa", [NT_A, D])
    xh = dt("xh", [34, D])
    ident_d = dt("ident", [128, 128])
    gmix_d = dt("gmix", [D])
    gffn_d = dt("gffn", [D])
    gkv_d = dt("gkv", [D])
    gckv_d = dt("gckv", [256])
    w_in_d = dt("w_in", [D, 3 * D])
    cw_d = dt("cw", [128, 8, 3])
    w_out_d = dt("w_out", [D, D])
    w_router_d = dt("w_router", [D, 72])
    wg_d = dt("wg", [NEXP, D, DEXP])
    wu_d = dt("wu", [NEXP, D, DEXP])
    wd_d = dt("wd", [NEXP, DEXP, D])
    w_dkvc_d = dt("w_dkvc", [D, 256])
    w_dkvA_d = dt("w_dkvA", [D, 64])
    w_dkvB_d = dt("w_dkvB", [D, 64])
    cosT_d = dt("cosT", [64, NT_A])
    sinT_d = dt("sinT", [64, NT_A])
    h1_d = dt("h1", [NT_A, D], F32, "ExternalOutput")
    latT_d = dt("latT", [320, NT_A], F32, "ExternalOutput")

    P = Prog(nc)
    C = Ctx()
    esP, sbP, psP = new_stack(nc)
    load_consts(P, C, sbP, ident_d)
    h = sbP("h", [128, NBLK + 1, D], F32)
    blocks = [(b, 128) for b in range(NBLK)] + [(NBLK, NMETA)]
    tok0 = [b * 128 for b in range(NBLK)] + [NTOK]

    esY, sbY, psY = new_stack(nc)
    yT = sbY("yT", [128, 8, NT_A], BF16)
    es, sb, ps = new_stack(nc)
    alloc_norm(C, sb, ps)
    w_in = sb("w_in", [128, 8, 3 * D], BF16)
    gbc = sb("gbc", [128, D], F32)
    cw = sb("cw", [128, 8, 3], F32)
    xt = [sb("xt%d" % i, [128, D], F32) for i in range(2)]
    hnT = [sb("hnT%d" % i, [128, 8, 512], BF16) for i in range(2)]
    zh = sb("zh", [128, 8, 34], F32)
    zext = [sb("zext%d" % i, [128, 4, 130], F32) for i in range(2)]
    csb = [sb("csb%d" % i, [128, 512], F32) for i in range(2)]
    acc = [sb("acc%d" % i, [128, 512], F32) for i in range(2)]
    pC = [ps("pC%d" % i, [128, 512], F32) for i in range(2)]
    pU = [ps("pU%d" % i, [128, 512], F32) for i in range(2)]
    pB = [ps("pB%d" % i, [128, 512], F32) for i in range(2)]
    for k in range(8):
        P.dma('pool', lambda e, k=k: e.dma_start(out=w_in[:, k, :].rearrange("p (a n) -> p a n", n=1024),
                                                  in_=w_in_d[k * 128:(k + 1) * 128, :].rearrange("p (a n) -> p a n", n=1024)),
              writes=[('w_in', k)], key=('w_in', k))
    load_bcast(P, 'sp', gbc, gmix_d, 'gbc')
    P.dma('sp', lambda e: e.dma_start(out=cw[:], in_=cw_d), writes=['cw'], key='cw')
    w_in_tags = [('w_in', k) for k in range(8)]

    def conv_group(gi, hnT_t, hn_tag, gn, nb, bs, halo0, tokbase, halo_only=False):
        for j in range(8):
            sl = (gi * 8 + j) % 2
            which = [('c', pC[sl], 8 + j), ('u', pU[sl], 16 + j)]
            if not halo_only:
                which.append(('b', pB[sl], j))
            for nm, pt, oc in which:
                for k in range(8):
                    P.op('pe', lambda e, pt=pt, oc=oc, k=k: e.matmul(pt[:, :gn], lhsT=w_in[:, k, oc * 128:(oc + 1) * 128], rhs=hnT_t[:, k, :gn],
                                                                      start=(k == 0), stop=(k == 7)),
                         reads=[hn_tag, ('w_in', k)], writes=[('p' + nm, sl)])
            P.op('act', lambda e, sl=sl: e.activation(out=csb[sl][:, :gn], in_=pC[sl][:, :gn], func=AF.Copy), reads=[('pc', sl)], writes=[('csb', sl)])
            if halo_only:
                P.op('dve', lambda e, sl=sl, j=j: e.tensor_tensor(out=zh[:, j, :gn], in0=csb[sl][:, :gn], in1=pU[sl][:, :gn], op=ALU.mult),
                     reads=[('csb', sl), ('pu', sl)], writes=[('zh', j)])
                continue
            ze = zext[sl]
            P.op('dve', lambda e, sl=sl, ze=ze: e.tensor_tensor(out=ze[:, :nb, 2:2 + bs], in0=csb[sl][:, :gn].rearrange("p (b t) -> p b t", t=bs),
                                                                 in1=pU[sl][:, :gn].rearrange("p (b t) -> p b t", t=bs), op=ALU.mult),
                 reads=[('csb', sl), ('pu', sl)], writes=[('zext', sl)])
            P.op('dve', lambda e, ze=ze, j=j: e.tensor_copy(out=ze[:, :nb, 0:2], in_=zh[:, j, 2 * halo0:2 * (halo0 + nb)].rearrange("p (b t) -> p b t", t=2)),
                 reads=[('zh', j)], writes=[('zext', sl)])
            av = acc[sl][:, :gn].rearrange("p (b t) -> p b t", t=bs)
            P.op('dve', lambda e, ze=ze, av=av, j=j: e.tensor_scalar(out=av, in0=ze[:, :nb, 2:2 + bs], scalar1=cw[:, j, 2:3], scalar2=None, op0=ALU.mult),
                 reads=[('zext', sl), 'cw'], writes=[('acc', sl)])
            P.op('dve', lambda e, ze=ze, av=av, j=j: e.scalar_tensor_tensor(out=av, in0=ze[:, :nb, 1:1 + bs], scalar=cw[:, j, 1:2], in1=av, op0=ALU.mult, op1=ALU.add),
                 reads=[('zext', sl), 'cw'], writes=[('acc', sl)])
            P.op('dve', lambda e, ze=ze, av=av, j=j: e.scalar_tensor_tensor(out=av, in0=ze[:, :nb, 0:bs], scalar=cw[:, j, 0:1], in1=av, op0=ALU.mult, op1=ALU.add),
                 reads=[('zext', sl), 'cw'], writes=[('acc', sl)])
            P.op('dve', lambda e, sl=sl, j=j: e.tensor_tensor(out=yT[:, j, tokbase:tokbase + gn], in0=acc[sl][:, :gn], in1=pB[sl][:, :gn], op=ALU.mult),
                 reads=[('acc', sl), ('pb', sl)], writes=[('yT', gi)])

    P.dma('sp', lambda e: e.dma_start(out=xt[0][:34, :], in_=xh), writes=[('xt', 0)], key=('xt', 0))
    norm_T(P, C, xt[0][:34, :], ('xt', 0), 34, gbc, 'gbc', dst16=hnT[0][:, :, :34], dst16_tag=('hnT', 0))
    conv_group(99, hnT[0], ('hnT', 0), 34, 17, 2, 0, 0, halo_only=True)
    xi = 1
    for gi in range(5):
        hs = (gi + 1) % 2
        if gi < 4:
            for j in range(4):
                b = gi * 4 + j
                s = xi % 2
                xi += 1
                P.dma('sp', lambda e, s=s, b=b: e.dma_start(out=xt[s][:, :], in_=xa[b * 128:(b + 1) * 128, :]), writes=[('xt', s)], key=('xt', s))
                norm_T(P, C, xt[s][:, :], ('xt', s), 128, gbc, 'gbc', dst16=hnT[hs][:, :, j * 128:(j + 1) * 128], dst16_tag=('hnT', hs))
            conv_group(gi, hnT[hs], ('hnT', hs), 512, 4, 128, gi * 4, gi * 512)
        else:
            s = xi % 2
            xi += 1
            P.dma('sp', lambda e, s=s: e.dma_start(out=xt[s][:NMETA, :], in_=xa[NTOK:NT_A, :]), writes=[('xt', s)], key=('xt', s))
            norm_T(P, C, xt[s][:NMETA, :], ('xt', s), NMETA, gbc, 'gbc', dst16=hnT[hs][:, :, :NMETA], dst16_tag=('hnT', hs))
            conv_group(gi, hnT[hs], ('hnT', hs), NMETA, 1, NMETA, 16, NTOK)
    P.emit()
    es.close()
    if stop_after <= 1:
        P.emit(final=True); esY.close(); esP.close(); return nc

    es, sb, ps = new_stack(nc)
    w_out = sb("w_out", [128, 8, D], BF16)
    po = [ps("po%d" % i, [128, 512], F32) for i in range(4)]
    P.dma('pool', lambda e: e.dma_start(out=w_out[:], in_=w_out_d.rearrange("(k p) n -> p k n", p=128)), writes=['w_out'], key='w_out')
    oi = 0
    for bi, (b, n) in enumerate(blocks):
        t0 = tok0[bi]
        P.dma('sp', lambda e, b=b, n=n, t0=t0: e.dma_start(out=h[:n, b, :], in_=xa[t0:t0 + n, :]), writes=[('h', b)], key=('h', b))
        for half in range(2):
            pi = oi % 4
            oi += 1
            for k in range(8):
                P.op('pe', lambda e, k=k, n=n, t0=t0, half=half, pi=pi: e.matmul(po[pi][:n, :], lhsT=yT[:, k, t0:t0 + n], rhs=w_out[:, k, half * 512:(half + 1) * 512],
                                                                              start=(k == 0), stop=(k == 7)),
                     reads=['w_out'], writes=[('po', pi)])
            P.op('dve', lambda e, b=b, n=n, half=half, pi=pi: e.tensor_tensor(out=h[:n, b, half * 512:(half + 1) * 512], in0=po[pi][:n, :],
                                                                             in1=h[:n, b, half * 512:(half + 1) * 512], op=ALU.add),
                 reads=[('po', pi), ('h', b)], writes=[('h', b)])
    P.emit()
    es.close()
    esY.close()
    def dump_h():
        for bi, (b, n) in enumerate(blocks):
            t0 = tok0[bi]
            P.dma('sp', lambda e, b=b, n=n, t0=t0: e.dma_start(out=h1_d[t0:t0 + n, :], in_=h[:n, b, :]), reads=[('h', b)], key='h1out')
    if stop_after <= 2:
        dump_h(); P.emit(final=True); esP.close(); return nc

    if nexp >= 0:
        moe_phase(P, nc, C, h, blocks, w_router_d, gffn_d, wg_d, wu_d, wd_d, nexp=nexp)
    if stop_after <= 3:
        dump_h(); P.emit(final=True); esP.close(); return nc

    es, sb, ps = new_stack(nc)
    alloc_norm(C, sb, ps)
    gbc = sb("gbc3", [128, D], F32)
    gck = sb("gck", [128, 256], F32)
    wc = sb("wc", [128, 8, 256], BF16)
    wA = sb("wA", [128, 8, 64], BF16)
    wB = sb("wB", [128, 8, 64], BF16)
    cosT = sb("cosT", [64, NT_A], F32)
    sinT = sb("sinT", [64, NT_A], F32)
    knT = [sb("knT%d" % i, [128, 8, 128], BF16) for i in range(2)]
    ckv = sb("ckv", [128, 256], F32)
    latc = sb("latc", [128, 2, NT_A], F32)
    latr = sb("latr", [64, NT_A], F32)
    tA = sb("tA", [64, 128], F32)
    tB = sb("tB", [64, 128], F32)
    craw = sb("craw", [128, 256], F32)
    pc = ps("pc3", [128, 512], F32)
    pA = ps("pA3", [128, 512], F32)
    pB3 = ps("pB3", [128, 512], F32)
    pT2 = ps("pT2", [128, 4, 128], F32)
    load_bcast(P, 'sp', gbc, gkv_d, 'gbc3')
    load_bcast(P, 'sp', gck, gckv_d, 'gck')
    P.dma('pool', lambda e: e.dma_start(out=wc[:], in_=w_dkvc_d.rearrange("(k p) n -> p k n", p=128)), writes=['wc'], key='wc')
    P.dma('pool', lambda e: e.dma_start(out=wA[:], in_=w_dkvA_d.rearrange("(k p) n -> p k n", p=128)), writes=['wA'], key='wA')
    P.dma('pool', lambda e: e.dma_start(out=wB[:], in_=w_dkvB_d.rearrange("(k p) n -> p k n", p=128)), writes=['wB'], key='wB')
    P.dma('sp', lambda e: e.dma_start(out=cosT[:], in_=cosT_d), writes=['cosT'], key='cosT')
    P.dma('sp', lambda e: e.dma_start(out=sinT[:], in_=sinT_d), writes=['sinT'], key='sinT')
    for bi, (b, n) in enumerate(blocks):
        t0 = tok0[bi]
        s = bi % 2
        P.dma('sp', lambda e, b=b, n=n, t0=t0: e.dma_start(out=h1_d[t0:t0 + n, :], in_=h[:n, b, :]), reads=[('h', b)], key='h1out')
        norm_T(P, C, h[:n, b, :], ('h', b), n, gbc, 'gbc3', dst16=knT[s][:, :, :n], dst16_tag=('knT', s))
        for k in range(8):
            P.op('pe', lambda e, k=k, n=n, s=s: e.matmul(pc[:n, :256], lhsT=knT[s][:, k, :n], rhs=wc[:, k, :], start=(k == 0), stop=(k == 7)),
                 reads=[('knT', s), 'wc'], writes=['pc3'])
        for k in range(8):
            P.op('pe', lambda e, k=k, n=n, s=s: e.matmul(pA[:64, :n], lhsT=wA[:, k, :], rhs=knT[s][:, k, :n], start=(k == 0), stop=(k == 7)),
                 reads=[('knT', s), 'wA'], writes=['pA3'])
        for k in range(8):
            P.op('pe', lambda e, k=k, n=n, s=s: e.matmul(pB3[:64, :n], lhsT=wB[:, k, :], rhs=knT[s][:, k, :n], start=(k == 0), stop=(k == 7)),
                 reads=[('knT', s), 'wB'], writes=['pB3'])
        P.op('act', lambda e, n=n: e.activation(out=craw[:n, :], in_=pc[:n, :256], func=AF.Copy), reads=['pc3'], writes=['craw'])
        norm_rstd(P, C, craw[:n, :], n, 256, 'craw')
        P.op('dve', lambda e, n=n: e.scalar_tensor_tensor(out=ckv[:n, :], in0=craw[:n, :], scalar=C.rstd[:n, :], in1=gck[:n, :], op0=ALU.mult, op1=ALU.mult),
             reads=['craw', 'rstd', 'gck'], writes=['ckv'])
        for kc in range(2):
            P.op('pe', lambda e, kc=kc, n=n: e.transpose(pT2[:, kc, :n], ckv[:n, kc * 128:(kc + 1) * 128], C.ident32[:n, :n]),
                 reads=['ckv', 'ident32'], writes=['pT2'])
        P.op('act', lambda e, n=n, t0=t0: e.activation(out=latc[:, :, t0:t0 + n], in_=pT2[:, 0:2, :n], func=AF.Copy), reads=['pT2'], writes=['latc'])
        P.op('dve', lambda e, n=n, t0=t0: e.tensor_tensor(out=tA[:, :n], in0=pA[:64, :n], in1=cosT[:, t0:t0 + n], op=ALU.mult), reads=['pA3', 'cosT'], writes=['tA'])
        P.op('dve', lambda e, n=n, t0=t0: e.tensor_tensor(out=tB[:, :n], in0=pB3[:64, :n], in1=sinT[:, t0:t0 + n], op=ALU.mult), reads=['pB3', 'sinT'], writes=['tB'])
        P.op('dve', lambda e, n=n, t0=t0: e.tensor_tensor(out=latr[:, t0:t0 + n], in0=tA[:, :n], in1=tB[:, :n], op=ALU.add), reads=['tA', 'tB'], writes=['latr'])
    for kc in range(2):
        P.dma('sp', lambda e, kc=kc: e.dma_start(out=latT_d[kc * 128:(kc + 1) * 128, :], in_=latc[:, kc, :]), reads=['latc'], key='latout')
    P.dma('sp', lambda e: e.dma_start(out=latT_d[256:320, :], in_=latr[:, :]), reads=['latr'], key='latout')
    P.emit(final=True)
    es.close()
    esP.close()
    return nc


def build_B(nexp=NEXP):
    nc = bass.Bass("TRN2", target_bir_lowering=False)
    dt = lambda n, s, d=F32, k="ExternalInput": nc.dram_tensor(n, s, d, kind=k).ap()
    h1_d = dt("h1", [NTOK, D])
    latT_d = dt("latT", [320, TKEYS])
    ident_d = dt("ident", [128, 128])
    gmix_d = dt("gmix", [D])
    gffn_d = dt("gffn", [D])
    gfin_d = dt("gfin", [D])
    gcq_d = dt("gcq", [384])
    w_dq_d = dt("w_dq", [D, 384])
    w_qn_d = dt("w_qn", [384, NH * 128])
    w_qrA_d = dt("w_qrA", [384, NH * 128])
    w_qrB_d = dt("w_qrB", [384, NH * 128])
    w_uk_d = dt("w_uk", [256, NH * 128])
    w_uv_d = dt("w_uv", [256, NH * 128])
    w_o_d = dt("w_o", [D, D])
    cos2_d = dt("cos2", [128, NTOK])
    sin2_d = dt("sin2", [128, NTOK])
    mask_d = dt("mask", [128, 2, 2048])
    w_router_d = dt("w_router", [D, 72])
    wg_d = dt("wg", [NEXP, D, DEXP])
    wu_d = dt("wu", [NEXP, D, DEXP])
    wd_d = dt("wd", [NEXP, DEXP, D])
    out_d = dt("out", [NTOK, D], F32, "ExternalOutput")

    P = Prog(nc)
    C = Ctx()
    esP, sbP, psP = new_stack(nc)
    load_consts(P, C, sbP, ident_d)
    blocks = [(b, 128) for b in range(NBLK)]

    oT = sbP("oT", [128, NH, NTOK], BF16)
    esQ, sbQ, psQ = new_stack(nc)
    cqnT = sbQ("cqnT", [128, 3, NTOK], BF16)
    es, sb, ps = new_stack(nc)
    alloc_norm(C, sb, ps)
    gbc = sb("gbc", [128, D], F32)
    gcq = sb("gcq", [128, 384], F32)
    w_dq = sb("w_dq", [128, 8, 384], BF16)
    xt = [sb("xt%d" % i, [128, D], F32) for i in range(2)]
    hnT = [sb("hnT%d" % i, [128, 8, 128], BF16) for i in range(2)]
    cqn = sb("cqn", [128, 384], F32)
    cqraw = sb("cqraw", [128, 384], F32)
    pq = ps("pq", [128, 512], F32)
    pT3 = ps("pT3", [128, 4, 128], F32)
    load_bcast(P, 'sp', gbc, gmix_d, 'gbc')
    load_bcast(P, 'sp', gcq, gcq_d, 'gcq')
    P.dma('pool', lambda e: e.dma_start(out=w_dq[:], in_=w_dq_d.rearrange("(k p) n -> p k n", p=128)), writes=['w_dq'], key='w_dq')
    for b in range(NBLK):
        s = b % 2
        P.dma('sp', lambda e, s=s, b=b: e.dma_start(out=xt[s][:, :], in_=h1_d[b * 128:(b + 1) * 128, :]), writes=[('xt', s)], key=('xt', s))
        norm_T(P, C, xt[s][:, :], ('xt', s), 128, gbc, 'gbc', dst16=hnT[s][:, :, :], dst16_tag=('hnT', s))
        for k in range(8):
            P.op('pe', lambda e, k=k, s=s: e.matmul(pq[:, :384], lhsT=hnT[s][:, k, :], rhs=w_dq[:, k, :], start=(k == 0), stop=(k == 7)),
                 reads=[('hnT', s), 'w_dq'], writes=['pq'])
        P.op('act', lambda e: e.activation(out=cqraw[:, :], in_=pq[:, :384], func=AF.Copy), reads=['pq'], writes=['cqraw'])
        norm_rstd(P, C, cqraw[:, :], 128, 384, 'cqraw')
        P.op('dve', lambda e: e.scalar_tensor_tensor(out=cqn[:, :], in0=cqraw[:, :], scalar=C.rstd[:, :], in1=gcq[:, :], op0=ALU.mult, op1=ALU.mult),
             reads=['cqraw', 'rstd', 'gcq'], writes=['cqn'])
        for kc in range(3):
            P.op('pe', lambda e, kc=kc: e.transpose(pT3[:, kc, :], cqn[:, kc * 128:(kc + 1) * 128], C.ident32[:, :]),
                 reads=['cqn', 'ident32'], writes=['pT3'])
        P.op('act', lambda e, b=b: e.activation(out=cqnT[:, :, b * 128:(b + 1) * 128], in_=pT3[:, 0:3, :], func=AF.Copy), reads=['pT3'], writes=['cqnT'])
    P.emit()
    es.close()

    es, sb, ps = new_stack(nc)
    wq_h = [sb("wq_h%d" % i, [128, 3, 3, 128], BF16) for i in range(2)]
    wkv_h = [sb("wkv_h%d" % i, [128, 2, 2, 128], BF16) for i in range(2)]
    cosg = [sb("cosg%d" % i, [128, 512], F32) for i in range(2)]
    sing = [sb("sing%d" % i, [128, 512], F32) for i in range(2)]
    maskb = sb("maskb", [128, 2, 2048], BF16)
    KR_HALF = 8192
    kr = sb("kr", [128, TKEYS - KR_HALF], BF16)
    KT = sb("KT", [128, TKEYS], BF16)
    NKB = 129
    V = sb("V", [128, NKB, 128], BF16)
    qnT = sb("qnT", [128, NTOK], BF16)
    qrT = sb("qrT", [128, NTOK], BF16)
    lat = [sb("lat%d" % i, [128, 2, 512], BF16) for i in range(3)]
    tq1 = sb("tq1", [128, 512], F32)
    tq2 = sb("tq2", [128, 512], F32)
    Pt = [sb("Pt%d" % i, [128, 512], BF16) for i in range(3)]
    PT = [sb("PT%d" % i, [128, 4, 128], BF16) for i in range(3)]
    rs = sb("rsum", [128, 2, 40], F32)
    rtot = sb("rtot", [128, 1], F32)
    rinv = sb("rinv", [128, 1], F32)
    osb = sb("osb", [128, 128], BF16)
    pS = [ps("pS%d" % i, [128, 512], F32) for i in range(2)]
    pPT = [ps("pPT%d" % i, [128, 8, 128], BF16) for i in range(2)]
    pO = [ps("pO%d" % i, [128, 512], F32) for i in range(2)]
    pX = [ps("pX%d" % i, [128, 512], F32) for i in range(2)]
    P.dma('pool', lambda e: e.dma_start(out=maskb[:], in_=mask_d), writes=['maskb'], key='maskb')
    for c0 in range(0, KR_HALF, 2048):
        P.dma('pool', lambda e, c0=c0: e.dma_start(out=kr[0:64, c0:c0 + 2048].rearrange("p (a n) -> p a n", n=1024),
                                                    in_=latT_d[256:320, c0:c0 + 2048].rearrange("p (a n) -> p a n", n=1024)), writes=['kr'], key='kr')
    n2 = TKEYS - KR_HALF
    for c0 in range(0, n2, 2048):
        cn = min(2048, n2 - c0)
        if cn == 2048:
            P.dma('pool', lambda e, c0=c0: e.dma_start(out=kr[64:128, c0:c0 + 2048].rearrange("p (a n) -> p a n", n=1024),
                                                        in_=latT_d[256:320, KR_HALF + c0:KR_HALF + c0 + 2048].rearrange("p (a n) -> p a n", n=1024)), writes=['kr'], key='kr')
        else:
            P.dma('pool', lambda e, c0=c0, cn=cn: e.dma_start(out=kr[64:128, c0:c0 + cn], in_=latT_d[256:320, KR_HALF + c0:KR_HALF + c0 + cn]), writes=['kr'], key='kr')

    NCH = 33
    li = 0
    xi = 0
    cp = 0

    def evac(out_ap, in_ap, reads, writes):
        nonlocal cp
        cp += 1
        if cp % 2 == 0:
            P.op('act', lambda e: e.activation(out=out_ap, in_=in_ap, func=AF.Copy), reads=reads, writes=writes)
        else:
            P.op('dve', lambda e: e.tensor_copy(out=out_ap, in_=in_ap), reads=reads, writes=writes)

    for hd in range(NH):
        hs = slice(hd * 128, (hd + 1) * 128)
        ws = hd % 2
        for wi, dd in enumerate((w_qn_d, w_qrA_d, w_qrB_d)):
            P.dma('pool', lambda e, wi=wi, dd=dd, ws=ws, hs=hs: e.dma_start(out=wq_h[ws][:, wi, :, :], in_=dd[:, hs].rearrange("(k p) n -> p k n", p=128)),
                  writes=[('wq_h', ws)], key=('wq_h', ws))
        for wi, dd in enumerate((w_uk_d, w_uv_d)):
            P.dma('pool', lambda e, wi=wi, dd=dd, ws=ws, hs=hs: e.dma_start(out=wkv_h[ws][:, wi, :, :], in_=dd[:, hs].rearrange("(k p) n -> p k n", p=128)),
                  writes=[('wkv_h', ws)], key=('wkv_h', ws))
        for g in range(4):
            ts_ = slice(g * 512, (g + 1) * 512)
            cs = (hd * 4 + g) % 2
            P.dma('sp', lambda e, cs=cs, ts_=ts_: e.dma_start(out=cosg[cs][:, :], in_=cos2_d[:, ts_]), writes=[('cosg', cs)], key=('cosg', cs))
            P.dma('sp', lambda e, cs=cs, ts_=ts_: e.dma_start(out=sing[cs][:, :], in_=sin2_d[:, ts_]), writes=[('sing', cs)], key=('sing', cs))
            px = pX[xi % 2]; ptag = ('pX', xi % 2); xi += 1
            for kc in range(3):
                P.op('pe', lambda e, kc=kc, px=px, ts_=ts_, ws=ws: e.matmul(px[:, :], lhsT=wq_h[ws][:, 0, kc, :], rhs=cqnT[:, kc, ts_], start=(kc == 0), stop=(kc == 2)),
                     reads=[('wq_h', ws), 'cqnT'], writes=[ptag])
            evac(qnT[:, ts_], px[:, :], [ptag], [('qnT', g)])
            px = pX[xi % 2]; ptag = ('pX', xi % 2); xi += 1
            for kc in range(3):
                P.op('pe', lambda e, kc=kc, px=px, ts_=ts_, ws=ws: e.matmul(px[:, :], lhsT=wq_h[ws][:, 1, kc, :], rhs=cqnT[:, kc, ts_], start=(kc == 0), stop=(kc == 2)),
                     reads=[('wq_h', ws), 'cqnT'], writes=[ptag])
            P.op('dve', lambda e, px=px, cs=cs: e.tensor_tensor(out=tq1[:, :], in0=px[:, :], in1=cosg[cs][:, :], op=ALU.mult), reads=[ptag, ('cosg', cs)], writes=['tq1'])
            px = pX[xi % 2]; ptag = ('pX', xi % 2); xi += 1
            for kc in range(3):
                P.op('pe', lambda e, kc=kc, px=px, ts_=ts_, ws=ws: e.matmul(px[:, :], lhsT=wq_h[ws][:, 2, kc, :], rhs=cqnT[:, kc, ts_], start=(kc == 0), stop=(kc == 2)),
                     reads=[('wq_h', ws), 'cqnT'], writes=[ptag])
            P.op('dve', lambda e, px=px, cs=cs: e.tensor_tensor(out=tq2[:, :], in0=px[:, :], in1=sing[cs][:, :], op=ALU.mult), reads=[ptag, ('sing', cs)], writes=['tq2'])
            P.op('dve', lambda e, ts_=ts_: e.tensor_tensor(out=qrT[:, ts_], in0=tq1[:, :], in1=tq2[:, :], op=ALU.add), reads=['tq1', 'tq2'], writes=[('qrT', g)])
        for ci in range(NCH):
            c0 = ci * 512
            cn = min(512, TKEYS - c0)
            ls = li % 3
            li += 1
            if cn == 512:
                P.dma('pool', lambda e, ls=ls, c0=c0: e.dma_start(out=lat[ls][:, :, :], in_=latT_d[0:256, c0:c0 + 512].rearrange("(k p) n -> p k n", p=128)),
                      writes=[('lat', ls)], key=('lat', ls))
            else:
                P.dma('pool', lambda e, ls=ls, c0=c0, cn=cn: e.dma_start(out=lat[ls][:, :, :cn], in_=latT_d[0:256, c0:c0 + cn].rearrange("(k p) n -> p k n", p=128)),
                      writes=[('lat', ls)], key=('lat', ls))
            px = pX[xi % 2]; ptag = ('pX', xi % 2); xi += 1
            for kc in range(2):
                P.op('pe', lambda e, kc=kc, px=px, ls=ls, cn=cn, ws=ws: e.matmul(px[:, :cn], lhsT=wkv_h[ws][:, 0, kc, :], rhs=lat[ls][:, kc, :cn], start=(kc == 0), stop=(kc == 1)),
                     reads=[('wkv_h', ws), ('lat', ls)], writes=[ptag])
            evac(KT[:, c0:c0 + cn], px[:, :cn], [ptag], [('KT', ci)])
            px = pX[xi % 2]; ptag = ('pX', xi % 2); xi += 1
            nbk = (cn + 127) // 128
            bs = min(128, cn)
            for i in range(nbk):
                for kc in range(2):
                    P.op('pe', lambda e, kc=kc, px=px, ls=ls, i=i, bs=bs, ws=ws: e.matmul(px[:bs, i * 128:(i + 1) * 128], lhsT=lat[ls][:, kc, i * 128:i * 128 + bs], rhs=wkv_h[ws][:, 1, kc, :],
                                                                                      start=(kc == 0), stop=(kc == 1)),
                         reads=[('wkv_h', ws), ('lat', ls)], writes=[ptag])
            evac(V[:bs, ci * 4:ci * 4 + nbk, :], px[:bs, :nbk * 128].rearrange("p (b d) -> p b d", d=128), [ptag], [('V', ci)])
        units = []
        for l in range(NBLK):
            m, s = l // 2, l % 2
            ul = [('meta', 32, None)]
            for ci in range(4 * m):
                ul.append(('plain', ci, None))
            for j4 in range(4):
                ul.append(('mask', 4 * m + j4, j4))
            for ui, (kind, ci, j4) in enumerate(ul):
                units.append(dict(l=l, s=s, kind=kind, ci=ci, j4=j4, ui=ui, first=(ui == 0), last=(ui == len(ul) - 1), nu=len(ul)))

        def qk(u, n):
            l, ci = u['l'], u['ci']
            qs = slice(l * 128, (l + 1) * 128)
            g = l // 4
            sl = n % 2
            cn = 16 if u['kind'] == 'meta' else 512
            c0 = ci * 512
            masked = (u['kind'] == 'mask')
            P.op('pe', lambda e: e.matmul(pS[sl][:, :cn], lhsT=qnT[:, qs], rhs=KT[:, c0:c0 + cn], start=True, stop=False),
                 reads=[('qnT', g), ('KT', ci)], writes=[('pS', sl)])
            if c0 < KR_HALF:
                P.op('pe', lambda e: e.matmul(pS[sl][:, :cn], lhsT=qrT[0:64, qs], rhs=kr[0:64, c0:c0 + cn], start=False, stop=(not masked)),
                     reads=[('qrT', g), 'kr'], writes=[('pS', sl)])
            else:
                P.op('pe', lambda e: e.matmul(pS[sl][:, :cn], lhsT=qrT[64:128, qs], rhs=kr[64:128, c0 - KR_HALF:c0 - KR_HALF + cn], start=False, stop=(not masked)),
                     reads=[('qrT', g), 'kr'], writes=[('pS', sl)])
            if masked:
                j4 = u['j4']
                P.op('pe', lambda e: e.matmul(pS[sl][:, :cn], lhsT=C.identb[:, :], rhs=maskb[:, u['s'], j4 * 512:(j4 + 1) * 512], start=False, stop=True),
                     reads=['identb', 'maskb'], writes=[('pS', sl)])
            p3 = n % 3
            P.op('act', lambda e: e.activation(out=Pt[p3][:, :cn], in_=pS[sl][:, :cn], func=AF.Exp, scale=SCALE, accum_out=rs[:, l % 2, u['ui']:u['ui'] + 1]),
                 reads=[('pS', sl)], writes=[('Pt', p3), ('rs', l % 2, u['ui'])])

        def tr(u, n):
            cn = 16 if u['kind'] == 'meta' else 512
            p3 = n % 3
            sl = n % 2
            if cn == 16:
                P.op('pe', lambda e: e.transpose(pPT[sl][:16, 0, :], Pt[p3][:, :16], C.identb[:, :]), reads=[('Pt', p3), 'identb'], writes=[('pPT', sl)])
                P.op('dve', lambda e: e.tensor_copy(out=PT[p3][:16, 0, :], in_=pPT[sl][:16, 0, :]), reads=[('pPT', sl)], writes=[('PT', p3)])
            else:
                for i in range(4):
                    P.op('pe', lambda e, i=i: e.transpose(pPT[sl][:, i, :], Pt[p3][:, i * 128:(i + 1) * 128], C.identb[:, :]), reads=[('Pt', p3), 'identb'], writes=[('pPT', sl)])
                P.op('dve', lambda e: e.tensor_copy(out=PT[p3][:, :, :], in_=pPT[sl][:, 0:4, :]), reads=[('pPT', sl)], writes=[('PT', p3)])

        def pv(u, n):
            l, ci = u['l'], u['ci']
            cn = 16 if u['kind'] == 'meta' else 512
            p3 = n % 3
            ol = l % 2
            if cn == 16:
                P.op('pe', lambda e: e.matmul(pO[ol][:, :128], lhsT=PT[p3][:16, 0, :], rhs=V[:16, 128, :], start=u['first'], stop=u['last']),
                     reads=[('PT', p3), ('V', 32)], writes=[('pO', ol)])
            else:
                for i in range(4):
                    P.op('pe', lambda e, i=i: e.matmul(pO[ol][:, :128], lhsT=PT[p3][:, i, :], rhs=V[:, ci * 4 + i, :], start=(u['first'] and i == 0), stop=(u['last'] and i == 3)),
                         reads=[('PT', p3), ('V', ci)], writes=[('pO', ol)])
            if u['last']:
                nu = u['nu']
                P.op('dve', lambda e: e.tensor_reduce(out=rtot[:, :], in_=rs[:, ol, :nu], axis=AX.X, op=ALU.add),
                     reads=[('rs', ol, i) for i in range(nu)], writes=['rtot'])
                P.op('dve', lambda e: e.reciprocal(out=rinv[:, :], in_=rtot[:, :]), reads=['rtot'], writes=['rinv'])
                P.op('dve', lambda e: e.tensor_scalar(out=osb[:, :], in0=pO[ol][:, :128], scalar1=rinv[:, :], scalar2=None, op0=ALU.mult),
                     reads=[('pO', ol), 'rinv'], writes=['osb'])
                pend.append(l)

        def fin(l, hd=hd):
            P.op('pe', lambda e: e.transpose(pPT[1][:, 4, :], osb[:, :], C.identb[:, :]), reads=['osb', 'identb'], writes=[('pPT', 1)])
            P.op('act', lambda e: e.activation(out=oT[:, hd, l * 128:(l + 1) * 128], in_=pPT[1][:, 4, :], func=AF.Copy), reads=[('pPT', 1)], writes=[('oT', hd, l)])

        pend = []
        nU = len(units)
        qk(units[0], 0)
        for n in range(nU):
            if n + 1 < nU:
                qk(units[n + 1], n + 1)
            tr(units[n], n)
            if n >= 1:
                pv(units[n - 1], n - 1)
                while pend:
                    fin(pend.pop(0))
        pv(units[nU - 1], nU - 1)
        while pend:
            fin(pend.pop(0))
    P.emit()
    es.close()
    esQ.close()

    h = sbP("h", [128, NBLK, D], F32)
    es, sb, ps = new_stack(nc)
    w_o = sb("w_o", [128, 8, D], BF16)
    po = [ps("po%d" % i, [128, 512], F32) for i in range(4)]
    P.dma('pool', lambda e: e.dma_start(out=w_o[:], in_=w_o_d.rearrange("(k p) n -> p k n", p=128)), writes=['w_o'], key='w_o')
    oi = 0
    for b in range(NBLK):
        P.dma('sp', lambda e, b=b: e.dma_start(out=h[:, b, :], in_=h1_d[b * 128:(b + 1) * 128, :]), writes=[('h', b)], key=('h', b))
        for half in range(2):
            pi = oi % 4
            oi += 1
            for k in range(8):
                P.op('pe', lambda e, k=k, b=b, half=half, pi=pi: e.matmul(po[pi][:, :], lhsT=oT[:, k, b * 128:(b + 1) * 128], rhs=w_o[:, k, half * 512:(half + 1) * 512],
                                                                        start=(k == 0), stop=(k == 7)),
                     reads=['w_o'], writes=[('po', pi)])
            P.op('dve', lambda e, b=b, half=half, pi=pi: e.tensor_tensor(out=h[:, b, half * 512:(half + 1) * 512], in0=po[pi][:, :],
                                                                        in1=h[:, b, half * 512:(half + 1) * 512], op=ALU.add),
                 reads=[('po', pi), ('h', b)], writes=[('h', b)])
    P.emit()
    es.close()

    moe_phase(P, nc, C, h, blocks, w_router_d, gffn_d, wg_d, wu_d, wd_d, nexp=nexp)

    es, sb, ps = new_stack(nc)
    alloc_norm(C, sb, ps)
    gbc = sb("gbc5", [128, D], F32)
    ot = [sb("ot%d" % i, [128, D], F32) for i in range(2)]
    load_bcast(P, 'sp', gbc, gfin_d, 'gbc5')
    for b in range(NBLK):
        s = b % 2
        norm_rstd(P, C, h[:, b, :], 128, D, ('h', b))
        P.op('dve', lambda e, b=b, s=s: e.scalar_tensor_tensor(out=ot[s][:, :], in0=h[:, b, :], scalar=C.rstd[:, :], in1=gbc[:, :], op0=ALU.mult, op1=ALU.mult),
             reads=[('h', b), 'rstd', 'gbc5'], writes=[('ot', s)])
        P.dma('sp', lambda e, b=b, s=s: e.dma_start(out=out_d[b * 128:(b + 1) * 128, :], in_=ot[s][:, :]), reads=[('ot', s)], key=('oto', s))
    P.emit(final=True)
    es.close()
    esP.close()
    return nc


def core_blocks(c):
    out = []
    for m in range(8):
        out.append(16 * m + c)
        out.append(16 * m + 15 - c)
    return out


def rope_tables(pos):
    inv = (10000.0 ** (-np.arange(0, 64, 2, dtype=np.float32) / np.float32(64))).astype(np.float32)
    ang = pos.astype(np.float32)[:, None] * inv[None, :]
    cos = np.cos(ang).astype(np.float32)
    sin = np.sin(ang).astype(np.float32)
    cosT = np.concatenate([cos, cos], axis=1).T.copy()
    sinT = np.concatenate([-sin, sin], axis=1).T.copy()
    return np.ascontiguousarray(cosT), np.ascontiguousarray(sinT)


_NC_CACHE = {}
_DBG = {}


def kernel(x, meta_tokens, norm_mix_g, norm_ffn_g, conv_w_in, conv_w, conv_w_out,
           kv_norm_g, w_dkv, ckv_norm_g, w_ukv, w_dq, cq_norm_g, w_uq, w_o,
           w_grp, w_exp, w_gate, w_up, w_down, final_norm_g):
    f = lambda a: np.ascontiguousarray(np.asarray(a, dtype=np.float32))
    x = f(x)[0]
    meta = f(meta_tokens)
    ident = np.eye(128, dtype=np.float32)
    w_dkv = f(w_dkv)
    w_uq = f(w_uq)[0].reshape(384, NH, 192)
    w_ukv = f(w_ukv).reshape(256, NH, 256)
    sw = np.concatenate([np.arange(32, 64), np.arange(0, 32)])
    if 'A' not in _NC_CACHE:
        _NC_CACHE['A'] = build_A()
    ncA = _NC_CACHE['A']
    cw = f(conv_w)[0, :, 0, :].reshape(3, 8, 128).transpose(2, 1, 0)
    common_A = dict(
        ident=ident, gmix=f(norm_mix_g)[0], gffn=f(norm_ffn_g)[0], gkv=f(kv_norm_g), gckv=f(ckv_norm_g),
        w_in=f(conv_w_in)[0], cw=f(cw), w_out=f(conv_w_out)[0],
        w_router=f(np.concatenate([f(w_grp)[0], f(w_exp)[0]], axis=1)),
        wg=f(w_gate)[0], wu=f(w_up)[0], wd=f(w_down)[0],
        w_dkvc=f(w_dkv[:, :256]), w_dkvA=f(w_dkv[:, 256:320]), w_dkvB=f(w_dkv[:, 256:320][:, sw]),
    )
    in_maps = []
    for c in range(NCORES):
        blks = core_blocks(c)
        rows = [x[g * 128:(g + 1) * 128] for g in blks] + [meta]
        xa = np.concatenate(rows, axis=0)
        halo = []
        for g in blks:
            halo.append(meta[14:16] if g == 0 else x[g * 128 - 2:g * 128])
        halo.append(np.zeros((2, D), np.float32))
        xh = np.concatenate(halo, axis=0)
        pos = np.concatenate([16 + g * 128 + np.arange(128) for g in blks] + [np.arange(16)])
        cosT, sinT = rope_tables(pos)
        d = dict(common_A)
        d.update(xa=f(xa), xh=f(xh), cosT=cosT, sinT=sinT)
        in_maps.append(d)
    resA = run_bass_kernel_spmd(ncA, in_maps, core_ids=list(range(NCORES)))
    latT = np.zeros((320, TKEYS), np.float32)
    h1s = []
    for c in range(NCORES):
        r = resA.results[c]
        lt = np.asarray(r["latT"], dtype=np.float32)
        for l, g in enumerate(core_blocks(c)):
            latT[:, g * 128:(g + 1) * 128] = lt[:, l * 128:(l + 1) * 128]
        if c == 0:
            latT[:, SEQ:] = lt[:, NTOK:]
        h1s.append(np.ascontiguousarray(np.asarray(r["h1"], dtype=np.float32)[:NTOK]))
    _DBG['latT'] = latT
    _DBG['h1s'] = h1s
    if 'B' not in _NC_CACHE:
        _NC_CACHE['B'] = build_B()
    ncB = _NC_CACHE['B']
    w_qn = f(w_uq[:, :, :128].reshape(384, NH * 128))
    qr = w_uq[:, :, 128:]
    w_qrA = f(np.concatenate([qr, qr], axis=2).reshape(384, NH * 128))
    qrs = qr[:, :, sw]
    w_qrB = f(np.concatenate([qrs, qrs], axis=2).reshape(384, NH * 128))
    common_B = dict(
        ident=ident, latT=latT, gmix=f(norm_mix_g)[1], gffn=f(norm_ffn_g)[1], gfin=f(final_norm_g), gcq=f(cq_norm_g)[0],
        w_dq=f(w_dq)[0], w_qn=w_qn, w_qrA=w_qrA, w_qrB=w_qrB,
        w_uk=f(w_ukv[:, :, :128].reshape(256, NH * 128)), w_uv=f(w_ukv[:, :, 128:].reshape(256, NH * 128)),
        w_o=f(w_o)[0],
        w_router=f(np.concatenate([f(w_grp)[1], f(w_exp)[1]], axis=1)),
        wg=f(w_gate)[1], wu=f(w_up)[1], wd=f(w_down)[1],
    )
    in_maps = []
    qi = np.arange(128)[:, None]
    ki = np.arange(128)[None, :]
    for c in range(NCORES):
        blks = core_blocks(c)
        pos = np.concatenate([16 + g * 128 + np.arange(128) for g in blks])
        cosT, sinT = rope_tables(pos)
        mask = np.zeros((128, 2, 2048), np.float32)
        for s, jsel in enumerate((c, 15 - c)):
            for j in range(16):
                if j > jsel:
                    mask[:, s, j * 128:(j + 1) * 128] = NEG
                elif j == jsel:
                    mask[:, s, j * 128:(j + 1) * 128] = np.where(ki <= qi, 0.0, NEG)
        d = dict(common_B)
        d.update(h1=h1s[c], cos2=f(np.concatenate([cosT, cosT], axis=0)), sin2=f(np.concatenate([sinT, sinT], axis=0)), mask=mask)
        in_maps.append(d)
    resB = run_bass_kernel_spmd(ncB, in_maps, core_ids=list(range(NCORES)))
    out = np.zeros((1, SEQ, D), np.float32)
    for c in range(NCORES):
        o = np.asarray(resB.results[c]["out"], dtype=np.float32)
        for l, g in enumerate(core_blocks(c)):
            out[0, g * 128:(g + 1) * 128] = o[l * 128:(l + 1) * 128]
    return out
```
